# Optimizing a Trainium2 kernel written in Bass

```python
import math
import jax, jax.numpy as jnp
from jax import lax
import numpy as np

D_MODEL = 1024
BATCH = 16
SEQ = 2048
DEPTH = 1

DN_HEADS = 4
DN_DK = 128
DN_DV = 128
CONV_K = 4
CHUNK = 64
DSA_HEADS = 8
DSA_DH = 64
Q_RANK = 256
KV_RANK = 128
IDX_HEADS = 8
IDX_DIM = 64
TOPK_MAX = 256
Q_BLOCK = 128
NUM_BUCKETS = 32
MAX_DISTANCE = 128
MEM_LEN = 256
X_HEADS = 4
X_DH = D_MODEL // X_HEADS
D_FF = -(-8 * D_MODEL // (3 * 256)) * 256
EPS = 1e-6
DN_QK_W = DN_HEADS * DN_DK
DN_V_W = DN_HEADS * DN_DV
PROJ_SIZES = (DN_QK_W, DN_QK_W, DN_V_W, DN_V_W, DN_HEADS, DN_HEADS, Q_RANK, KV_RANK, IDX_DIM, IDX_HEADS)
PROJ_OUT = sum(PROJ_SIZES)
MIX_WIDTH = DN_V_W + DSA_HEADS * DSA_DH

kernel_name = 'hybrid_gdn_dsa_block'


def rmsnorm(x, g):
    xf = x.astype(jnp.float32)
    y = xf * lax.rsqrt(jnp.mean(xf * xf, axis=-1, keepdims=True) + EPS)
    return (y * g.astype(jnp.float32)).astype(x.dtype)


def layernorm(x, g, b):
    xf = x.astype(jnp.float32)
    mu = jnp.mean(xf, axis=-1, keepdims=True)
    xc = xf - mu
    y = xc * lax.rsqrt(jnp.mean(xc * xc, axis=-1, keepdims=True) + EPS)
    return (y * g.astype(jnp.float32) + b.astype(jnp.float32)).astype(x.dtype)


def l2norm(x):
    xf = x.astype(jnp.float32)
    return (xf * lax.rsqrt(jnp.sum(xf * xf, axis=-1, keepdims=True) + EPS)).astype(x.dtype)


def causal_depthwise_conv(x, w):
    width = w.shape[0]
    return lax.conv_general_dilated(
        x, w[:, None, :].astype(x.dtype), window_strides=(1,), padding=[(width - 1, 0)],
        dimension_numbers=('NWC', 'WIO', 'NWC'), feature_group_count=x.shape[-1])


def relative_bucket(dist):
    max_exact = NUM_BUCKETS // 2
    d = jnp.maximum(dist, 0)
    log_ratio = jnp.log(jnp.maximum(d, max_exact).astype(jnp.float32) / max_exact) / math.log(MAX_DISTANCE / max_exact)
    large = max_exact + (log_ratio * (NUM_BUCKETS - max_exact)).astype(jnp.int32)
    return jnp.where(d < max_exact, d, jnp.minimum(large, NUM_BUCKETS - 1))


def gated_delta_rule_chunked(q, k, v, beta, g):
    b, s, nh, dk = q.shape
    dv = v.shape[-1]
    nc = s // CHUNK
    f32 = jnp.float32

    def chunks(t):
        return jnp.moveaxis(t.astype(f32).reshape(b, nc, CHUNK, nh, *t.shape[3:]), 3, 1)

    q, k, v, beta, g = chunks(q), chunks(k), chunks(v), chunks(beta), chunks(g)
    g_cum = jnp.cumsum(g, axis=-1)
    pos = jnp.arange(CHUNK)
    incl = pos[:, None] >= pos[None, :]
    strict = pos[:, None] > pos[None, :]
    decay = jnp.exp(jnp.where(incl, g_cum[..., :, None] - g_cum[..., None, :], -jnp.inf))
    kk = jnp.einsum('bhncd,bhnmd->bhncm', k, k)
    a_mat = jnp.where(strict, beta[..., :, None] * kk * decay, 0.0) + jnp.eye(CHUNK, dtype=f32)
    rhs = jnp.concatenate([v * beta[..., None], k * (beta * jnp.exp(g_cum))[..., None]], axis=-1)
    sol = lax.linalg.triangular_solve(a_mat, rhs, left_side=True, lower=True, unit_diagonal=True)
    u, w = sol[..., :dv], sol[..., dv:]
    qk = jnp.where(incl, jnp.einsum('bhncd,bhnmd->bhncm', q, k) * decay, 0.0)
    q_dec = q * jnp.exp(g_cum)[..., None]
    k_dec = k * jnp.exp(g_cum[..., -1:] - g_cum)[..., None]
    last = jnp.exp(g_cum[..., -1])

    def step(state, xs):
        u_c, w_c, qk_c, qd_c, kd_c, last_c = xs
        v_new = u_c - jnp.einsum('bhcd,bhde->bhce', w_c, state)
        o_c = jnp.einsum('bhcd,bhde->bhce', qd_c, state) + jnp.einsum('bhcm,bhme->bhce', qk_c, v_new)
        state = state * last_c[..., None, None] + jnp.einsum('bhcd,bhce->bhde', kd_c, v_new)
        return state, o_c

    xs = tuple(jnp.moveaxis(t, 2, 0) for t in (u, w, qk, q_dec, k_dec, last))
    _, o = lax.scan(step, jnp.zeros((b, nh, dk, dv), f32), xs)
    return jnp.transpose(o, (1, 0, 3, 2, 4)).reshape(b, s, nh, dv)


def dsa_sparse_attention(q_abs, c_kv, q_idx, w_idx, k_idx, rel_bias):
    b, s = c_kv.shape[:2]
    k_sel = min(TOPK_MAX, s // 4)
    nb = s // Q_BLOCK
    key_pos = jnp.arange(s)

    def blocks(t):
        return jnp.moveaxis(t.reshape(b, nb, Q_BLOCK, *t.shape[2:]), 1, 0)

    def one_block(args):
        qi, wi, qa, tq = args
        rel = jax.nn.relu(jnp.einsum('bqhd,bsd->bqhs', qi, k_idx))
        score = jnp.einsum('bqh,bqhs->bqs', wi, rel).astype(jnp.float32)
        score = jnp.where(key_pos[None, None, :] <= tq[None, :, None], score, -jnp.inf)
        _, idx = lax.top_k(score, k_sel)
        c_sel = jax.vmap(lambda c, i: c[i])(c_kv, idx)
        dist = tq[None, :, None] - idx
        bias = jnp.moveaxis(rel_bias[relative_bucket(dist)], -1, 2)
        logits = jnp.einsum('bqhc,bqkc->bqhk', qa, c_sel).astype(jnp.float32) * (DSA_DH ** -0.5)
        logits = jnp.where((dist >= 0)[:, :, None, :], logits + bias.astype(jnp.float32), -1e30)
        p = jax.nn.softmax(logits, axis=-1).astype(c_sel.dtype)
        return jnp.einsum('bqhk,bqkc->bqhc', p, c_sel)

    t_blocks = jnp.arange(s).reshape(nb, Q_BLOCK)
    o = lax.map(one_block, (blocks(q_idx), blocks(w_idx), blocks(q_abs), t_blocks))
    return jnp.moveaxis(o, 0, 1).reshape(b, s, *q_abs.shape[2:])


def setup_inputs(seed: int = 0) -> dict:
    key = jax.random.key(seed)
    ks = jax.random.split(key, 32)
    L = DEPTH
    f32 = jnp.float32

    def nrm(k, shape, fan_in):
        return jax.random.normal(k, shape, f32) * (fan_in ** -0.5)

    def gain(k, shape):
        return 1.0 + 0.02 * jax.random.normal(k, shape, f32)

    dt = jnp.exp(jax.random.uniform(ks[5], (L, DN_HEADS), f32, math.log(1e-3), math.log(1e-1)))
    return {
        'x': jax.random.normal(ks[0], (BATCH, SEQ, D_MODEL), f32),
        'mem': jax.random.normal(ks[1], (BATCH, MEM_LEN, D_MODEL), f32),
        'g_mix': gain(ks[2], (L, D_MODEL)),
        'w_in': nrm(ks[3], (L, D_MODEL, PROJ_OUT), D_MODEL),
        'conv_w': nrm(ks[4], (L, CONV_K, 2 * DN_QK_W + DN_V_W), CONV_K),
        'a_log': jnp.log(jax.random.uniform(ks[6], (L, DN_HEADS), f32, 1.0, 16.0)),
        'dt_bias': dt + jnp.log(-jnp.expm1(-dt)),
        'dn_norm_g': gain(ks[7], (L, DN_DV)),
        'q_norm_g': gain(ks[8], (L, Q_RANK)),
        'kv_norm_g': gain(ks[9], (L, KV_RANK)),
        'w_uq': nrm(ks[10], (L, Q_RANK, DSA_HEADS * DSA_DH), Q_RANK),
        'w_uk': nrm(ks[11], (L, DSA_HEADS, DSA_DH, KV_RANK), DSA_DH),
        'w_uv': nrm(ks[12], (L, DSA_HEADS, KV_RANK, DSA_DH), KV_RANK),
        'w_qidx': nrm(ks[13], (L, Q_RANK, IDX_HEADS * IDX_DIM), Q_RANK),
        'kidx_ln_g': gain(ks[14], (L, IDX_DIM)),
        'kidx_ln_b': 0.02 * jax.random.normal(ks[15], (L, IDX_DIM), f32),
        'rel_bias': 0.5 * jax.random.normal(ks[16], (NUM_BUCKETS, DSA_HEADS), f32),
        'w_out': nrm(ks[17], (L, MIX_WIDTH, D_MODEL), MIX_WIDTH),
        'g_xattn': gain(ks[18], (L, D_MODEL)),
        'g_mem': gain(ks[19], (L, D_MODEL)),
        'w_xq': nrm(ks[20], (L, D_MODEL, D_MODEL), D_MODEL),
        'w_xkv': nrm(ks[21], (L, D_MODEL, 2 * D_MODEL), D_MODEL),
        'w_xo': nrm(ks[22], (L, D_MODEL, D_MODEL), D_MODEL),
        'g_ffn': gain(ks[23], (L, D_MODEL)),
        'w_gate': nrm(ks[24], (L, D_MODEL, D_FF), D_MODEL),
        'w_up': nrm(ks[25], (L, D_MODEL, D_FF), D_MODEL),
        'w_down': nrm(ks[26], (L, D_FF, D_MODEL), D_FF),
        'g_final': gain(ks[27], (D_MODEL,)),
    }


def reference(x, mem, g_mix, w_in, conv_w, a_log, dt_bias, dn_norm_g, q_norm_g, kv_norm_g, w_uq, w_uk, w_uv,
              w_qidx, kidx_ln_g, kidx_ln_b, rel_bias, w_out, g_xattn, g_mem, w_xq, w_xkv, w_xo, g_ffn,
              w_gate, w_up, w_down, g_final):
    b, s, _ = x.shape
    m_len = mem.shape[1]
    f32 = jnp.float32
    offs = np.cumsum(PROJ_SIZES)[:-1].tolist()
    for l in range(DEPTH):
        h = rmsnorm(x, g_mix[l])
        proj = h @ w_in[l]
        dq, dk, dv, dz, db, da, cq, ckv, kix, wix = jnp.split(proj, offs, axis=-1)

        qkv = jax.nn.silu(causal_depthwise_conv(jnp.concatenate([dq, dk, dv], axis=-1), conv_w[l]))
        dq, dk, dv = jnp.split(qkv, [DN_QK_W, 2 * DN_QK_W], axis=-1)
        q = l2norm(dq.reshape(b, s, DN_HEADS, DN_DK)) * (DN_DK ** -0.5)
        k = l2norm(dk.reshape(b, s, DN_HEADS, DN_DK))
        v = dv.reshape(b, s, DN_HEADS, DN_DV)
        beta = jax.nn.sigmoid(db.astype(f32))
        g = -jnp.exp(a_log[l].astype(f32)) * jax.nn.softplus(da.astype(f32) + dt_bias[l].astype(f32))
        o_dn = gated_delta_rule_chunked(q, k, v, beta, g).astype(x.dtype)
        o_dn = rmsnorm(o_dn, dn_norm_g[l]) * jax.nn.silu(dz).reshape(b, s, DN_HEADS, DN_DV)
        o_dn = o_dn.reshape(b, s, DN_V_W)

        cq = rmsnorm(cq, q_norm_g[l])
        q_d = (cq @ w_uq[l]).reshape(b, s, DSA_HEADS, DSA_DH)
        q_abs = jnp.einsum('bshd,hdc->bshc', q_d, w_uk[l])
        c_kv = rmsnorm(ckv, kv_norm_g[l])
        q_idx = (cq @ w_qidx[l]).reshape(b, s, IDX_HEADS, IDX_DIM)
        k_idx = layernorm(kix, kidx_ln_g[l], kidx_ln_b[l])
        w_idx = wix * (IDX_HEADS ** -0.5 * IDX_DIM ** -0.5)
        o_lat = dsa_sparse_attention(q_abs, c_kv, q_idx, w_idx, k_idx, rel_bias)
        o_dsa = jnp.einsum('bshc,hcd->bshd', o_lat, w_uv[l]).reshape(b, s, DSA_HEADS * DSA_DH)

        x = x + jnp.concatenate([o_dn, o_dsa], axis=-1) @ w_out[l]

        hx = rmsnorm(x, g_xattn[l])
        mn = rmsnorm(mem, g_mem[l])
        qx = (hx @ w_xq[l]).reshape(b, s, X_HEADS, X_DH)
        kvx = (mn @ w_xkv[l]).reshape(b, m_len, 2, X_HEADS, X_DH)
        logits = jnp.einsum('bshd,bmhd->bhsm', qx, kvx[:, :, 0]).astype(f32) * (X_DH ** -0.5)
        p = jax.nn.softmax(logits, axis=-1).astype(x.dtype)
        ox = jnp.einsum('bhsm,bmhd->bshd', p, kvx[:, :, 1]).reshape(b, s, D_MODEL)
        x = x + ox @ w_xo[l]

        hf = rmsnorm(x, g_ffn[l])
        x = x + (jax.nn.silu(hf @ w_gate[l]) * (hf @ w_up[l])) @ w_down[l]
    return rmsnorm(x, g_final)
```

```python
import os
import numpy as np
from contextlib import ExitStack
import concourse.bass as bass
import concourse.mybir as mybir
from concourse.bass_utils import run_bass_kernel_spmd

F32 = mybir.dt.float32
BF16 = mybir.dt.bfloat16
AF = mybir.ActivationFunctionType
ALU = mybir.AluOpType
AX = mybir.AxisListType


class Region:
    __slots__ = ("w", "r", "sem", "dma_w", "dma_all", "dma_tot")

    def __init__(self):
        self.w = None
        self.r = {}
        self.sem = None
        self.dma_w = 0
        self.dma_all = 0
        self.dma_tot = 0


class Tile:
    def __init__(self, h, name, nsub=1):
        self.h = h
        self.name = name
        self.regs = [Region() for _ in range(nsub)]

    def __getitem__(self, key):
        return self.h[key]

    def rg(self, a, b=None):
        return (self, a, a + 1 if b is None else b)


class DramDep:
    def __init__(self, sem):
        self.sem = sem
        self.tot = 0


class FW:
    ENG = ("pe", "act", "dve", "pool", "sp")

    def __init__(self, nc):
        self.nc = nc
        self.es = ExitStack()
        self.eng = {"pe": nc.tensor, "act": nc.scalar, "dve": nc.vector, "pool": nc.gpsimd, "sp": nc.sync}
        self.cnt = {e: 0 for e in self.ENG}
        self.pend = {e: 0 for e in self.ENG}
        self.seen = {e: {} for e in self.ENG}
        self.sem = {}
        self.scopes = []
        self.nsem = 0
        self.nalloc = 0
        self.live = []
        self.nwait = 0
        self.nops = 0

    def __enter__(self):
        self.es.__enter__()
        for e in ("pe", "act", "dve", "pool"):
            self.sem[e] = self.new_sem("c_" + e)
        self.free_sems = []
        self.final_regs = []
        self.all_dma = {}
        self.noreuse = set()
        self.shared = self.new_sem("setup")
        self.shared_tot = 0
        self.shared_regs = []
        return self

    def __exit__(self, *a):
        for t in reversed(self.live):
            t.guard.__exit__(None, None, None)
        self.live = []
        return self.es.__exit__(*a)

    def new_sem(self, name):
        self.nsem += 1
        return self.es.enter_context(self.nc.semaphore(name + "_%d" % self.nsem))

    def _stack(self):
        return self.scopes[-1][0] if self.scopes else self.es

    def sb(self, name, shape, dtype, nsub=1, side=None):
        self.nalloc += 1
        g = self.nc.sbuf_tensor("%s_%d" % (name, self.nalloc), list(shape), dtype, side=side)
        h = g.__enter__()
        t = Tile(h, name, nsub)
        t.guard = g
        self.live.append(t)
        return t

    def free(self, tiles):
        self.barrier(tiles)
        for t in reversed(list(tiles)):
            t.guard.__exit__(None, None, None)
            self.live.remove(t)

    def ps(self, name, shape, dtype, nsub=1):
        h = self.es.enter_context(self.nc.psum_tensor(name, list(shape), dtype))
        return Tile(h, name, nsub)

    def dram_dep(self, name):
        return DramDep(self.new_sem("d_" + name))

    class _Scope:
        def __init__(self, fw):
            self.fw = fw

        def __enter__(self):
            st = ExitStack()
            st.__enter__()
            self.fw.scopes.append((st, []))
            return self

        def __exit__(self, *a):
            st, tiles = self.fw.scopes.pop()
            self.fw.barrier(tiles)
            return st.__exit__(*a)

    def scope(self):
        return FW._Scope(self)

    def _wait(self, e, key, sem, val):
        if val <= 0:
            return
        s = self.seen[e]
        if s.get(key, 0) >= val:
            return
        s[key] = val
        self.eng[e].wait_ge(sem, val)
        self.nwait += 1

    def _wait_eng(self, e, dep):
        f, c = dep
        if f == "pe" and e == "pe":
            return
        assert c <= self.cnt[f], "wait on a not-yet-emitted signalling op (deadlock hazard): %s waits %s>=%d" % (e, f, c)
        self._wait(e, f, self.sem[f], c)

    def _regs(self, lst):
        out = []
        for x in lst or ():
            if isinstance(x, Tile):
                out.extend(x.regs)
            elif isinstance(x, Region):
                out.append(x)
            else:
                t, a, b = x
                out.extend(t.regs[a:b])
        return out

    def _deps(self, e, rr, ww):
        for g in rr:
            if g.w is not None:
                self._wait_eng(e, g.w)
            if g.sem is not None:
                self._wait(e, id(g), g.sem, g.dma_w)
        for g in ww:
            if g.w is not None:
                self._wait_eng(e, g.w)
            for f, c in g.r.items():
                self._wait_eng(e, (f, c))
            if g.sem is not None:
                self._wait(e, id(g), g.sem, g.dma_all)

    def op(self, e, fn, r=None, w=None, signal=True):
        rr = self._regs(r)
        ww = self._regs(w)
        self._deps(e, rr, ww)
        ins = fn(self.eng[e])
        self.nops += 1
        if signal:
            ins.then_inc(self.sem[e], 1)
            self.cnt[e] += 1
            c = self.cnt[e]
            self.pend[e] = 0
        else:
            c = self.cnt[e] + 1
            self.pend[e] += 1
        for g in rr:
            if g.r.get(e, 0) < c:
                g.r[e] = c
        for g in ww:
            g.w = (e, c)
            g.r = {}
        return ins

    def dma(self, q, out, in_, out_t=None, in_t=None, dram_in=None, dram_out=None, final=False, shared=False, **kw):
        ro = self._regs([out_t] if out_t is not None else [])
        ri = self._regs([in_t] if in_t is not None else [])
        self._deps(q, ri, ro)
        if dram_in is not None:
            for dd in (dram_in if isinstance(dram_in, (list, tuple)) else [dram_in]):
                self._wait(q, id(dd), dd.sem, dd.tot)
        ins = self.eng[q].dma_start(out=out, in_=in_, **kw)
        self.nops += 1
        if dram_out is not None:
            ins.then_inc(dram_out.sem, 16)
            dram_out.tot += 16
            self.all_dma[id(dram_out.sem)] = (dram_out.sem, dram_out.tot)
            self.noreuse.add(id(dram_out.sem))
            for g in ri:
                g.sem = dram_out.sem
                g.dma_tot = dram_out.tot
                g.dma_all = dram_out.tot
            return ins
        if shared:
            ins.then_inc(self.shared, 16)
            self.shared_tot += 16
            self.all_dma[id(self.shared)] = (self.shared, self.shared_tot)
            for g in ro:
                g.sem = self.shared
                g.w = None
                g.r = {}
                self.shared_regs.append(g)
            return ins
        regs = ro if ro else ri
        g0 = regs[0]
        if g0.sem is None or g0.sem is self.shared:
            if self.free_sems:
                sem, base = self.free_sems.pop()
            else:
                sem, base = self.new_sem("r"), 0
            for g in regs:
                g.sem = sem
                g.dma_tot = base
                g.dma_w = 0
                g.dma_all = 0
        if final:
            self.final_regs.extend(regs)
        sem = g0.sem
        ins.then_inc(sem, 16)
        tot = g0.dma_tot + 16
        self.all_dma[id(sem)] = (sem, tot)
        for g in regs:
            assert g.sem is sem
            g.dma_tot = tot
            g.dma_all = tot
            if ro:
                g.dma_w = tot
                g.w = None
                g.r = {}
        return ins

    def freeze_shared(self):
        for g in self.shared_regs:
            g.dma_w = self.shared_tot
            g.dma_all = self.shared_tot
        self.shared_regs = []

    def barrier(self, tiles=()):
        for e in self.ENG:
            for f in ("pe", "act", "dve", "pool"):
                if f != e:
                    assert self.pend[f] == 0, "barrier with pending unsignalled ops on " + f
                    self._wait(e, f, self.sem[f], self.cnt[f])
        for t in tiles:
            for g in t.regs:
                if g.sem is not None and g.dma_all > 0:
                    for e in self.ENG:
                        self._wait(e, id(g), g.sem, g.dma_all)
        done = set()
        for t in tiles:
            for g in t.regs:
                if g.sem is not None and g.sem is not self.shared and id(g.sem) not in done and id(g.sem) not in self.noreuse:
                    done.add(id(g.sem))
                    self.free_sems.append((g.sem, g.dma_tot))
                    g.sem = None

    def finish(self):
        self.barrier()
        for g in self.final_regs:
            if g.sem is not None:
                self._wait("sp", id(g), g.sem, g.dma_all)
        for k, (sem, tot) in self.all_dma.items():
            self.eng["sp"].wait_ge(sem, tot)


S = 2048
D = 1024
NT = S // 128
NB = 2
PROJ = 2512
DFF = 2816
NFT = DFF // 128
EPS = 1e-6
R_GMIX, R_GX, R_GF, R_GFIN, R_GMEM = 0, 1024, 2048, 3072, 4096
R_SMALL = 5120
R_QN, R_KVN, R_KIG, R_KIB, R_DNG, R_ALOG, R_DTB, R_RB31 = 0, 256, 384, 448, 512, 640, 644, 648
NR_SMALL = 656
NR = R_SMALL + NR_SMALL
C_ID, C_TRI, C_SU, C_SL, C_IU, C_CM, C_ONE, C_LI = 0, 128, 256, 384, 512, 640, 768, 896
NCONST = 1024
GQ = 4
NBIS = 14
TOPK = 256


CUT = 99


class _Stop(Exception):
    pass


def build_program(dbg=False, stop=99):
    nc = bass.Bass("TRN2", target_bir_lowering=False)

    def din(name, shape, dt=F32):
        return nc.dram_tensor(name, list(shape), dt, kind="ExternalInput").ap()

    x_d = din("x", [NB, S, D])
    mem_d = din("mem", [NB, 256, D])
    w_in_d = din("w_in", [D, PROJ])
    w_out_d = din("w_out", [D, D])
    w_xq_d = din("w_xq", [D, D])
    w_xkv_d = din("w_xkv", [D, 2 * D])
    w_xo_d = din("w_xo", [D, D])
    w_gate_d = din("w_gate", [D, DFF])
    w_up_d = din("w_up", [D, DFF])
    w_down_d = din("w_down", [DFF, D])
    w_qidx_d = din("w_qidx", [256, 512])
    rows_d = din("rows", [128, NR])
    consts_d = din("consts", [128, NCONST])
    convw_d = din("convw", [128, 48])
    bt_d = din("bt", [128, 8 * 256])
    w_uqT_d = din("w_uqT", [64, 8 * 256])
    w_ukd_d = din("w_ukd", [64, 8 * 128])
    w_uvT_d = din("w_uvT", [64, 8 * 128])
    w_outB_d = din("w_outB", [64, 8 * 1024])
    out_d = nc.dram_tensor("out", [NB, S, D], F32, kind="ExternalOutput").ap()

    def scratch(name, shape):
        return nc.dram_tensor(name, list(shape), BF16, kind="Internal").ap()

    w_in_b = scratch("w_in_b", [D, PROJ])
    w_mix_b = scratch("w_mix_b", [1536, D])
    w_xq_b = scratch("w_xq_b", [D, D])
    w_xkv_b = scratch("w_xkv_b", [D, 2 * D])
    w_xo_b = scratch("w_xo_b", [D, D])
    w_gate_b = scratch("w_gate_b", [D, DFF])
    w_up_b = scratch("w_up_b", [D, DFF])
    w_down_b = scratch("w_down_b", [DFF, D])
    w_qidx_b = scratch("w_qidx_b", [256, 512])

    dbg_outs = {}

    fw = FW(nc)
    with fw:
        op = fw.op

        def dump(name, ap, tile, shape):
            if not dbg:
                return
            if name in dbg_outs:
                return
            d = nc.dram_tensor("dbg_" + name, list(shape), ap.dtype, kind="ExternalOutput").ap()
            dbg_outs[name] = d
            fw.dma("sp", d, ap, in_t=tile, final=True)

        P = [fw.ps("P%d" % i, [128, 512], F32) for i in range(6)]
        PT = [fw.ps("PT%d" % i, [128, 1024], BF16) for i in range(2)]
        pctr = [0, 0]

        def pn(lo=0, hi=6):
            pctr[0] = (pctr[0] + 1) % (hi - lo)
            return P[lo + pctr[0]]

        def ptn():
            pctr[1] = (pctr[1] + 1) % 2
            return PT[pctr[1]]

        consts = fw.sb("consts", [128, NCONST], F32)
        rows_s = fw.sb("rows_s", [128, NR_SMALL], F32)
        idb = fw.sb("idb", [128, 128], BF16)
        onesb = fw.sb("onesb", [128, 128], BF16)
        cc = fw.sb("cc", [128, 8], F32)
        aneg = fw.sb("aneg", [128, 4], F32)
        nrb31 = fw.sb("nrb31", [128, 8], F32)
        slots = [fw.sb("slot%d" % i, [128, 8, 512], BF16) for i in range(2)]
        sctr = [0]

        def load_w(wb, dep, k0, nk, c0, ncol):
            sctr[0] = (sctr[0] + 1) % len(slots)
            sl = slots[sctr[0]]
            fw.dma("sp", sl[:, 0:nk, 0:ncol],
                   wb[k0 * 128:(k0 + nk) * 128, c0:c0 + ncol].rearrange("(k p) c -> p k c", p=128),
                   out_t=sl, dram_in=dep)
            return sl

        fw.dma("sp", consts[:], consts_d, out_t=consts, shared=True)
        fw.dma("sp", rows_s[:], rows_d[:, R_SMALL:NR], out_t=rows_s, shared=True)
        ident = consts[:, C_ID:C_ID + 128]
        tri = consts[:, C_TRI:C_TRI + 128]
        su = consts[:, C_SU:C_SU + 128]
        slm = consts[:, C_SL:C_SL + 128]
        ium = consts[:, C_IU:C_IU + 128]
        cmm = consts[:, C_CM:C_CM + 128]
        ones_f = consts[:, C_ONE:C_ONE + 128]
        lim = consts[:, C_LI:C_LI + 128]

        deps = {}

        def cast_w(name, dst, src, nrows):
            dp = fw.dram_dep(name)
            deps[name] = dp
            for r0 in range(0, nrows, 128):
                fw.dma("pool", dst[r0:r0 + 128, :], src[r0:r0 + 128, :], dram_out=dp)
            return dp

        cast_w("w_in", w_in_b, w_in_d, D)
        cast_w("w_qidx", w_qidx_b, w_qidx_d, 256)
        dmix = fw.dram_dep("w_mix")
        dmixB = fw.dram_dep("w_mixB")
        deps["w_mix"] = dmix
        for r0 in range(0, 512, 128):
            fw.dma("pool", w_mix_b[r0:r0 + 128, :], w_out_d[r0:r0 + 128, :], dram_out=dmix)
        cast_w("w_xkv", w_xkv_b, w_xkv_d, D)
        cast_w("w_xq", w_xq_b, w_xq_d, D)
        cast_w("w_xo", w_xo_b, w_xo_d, D)
        cast_w("w_gate", w_gate_b, w_gate_d, D)
        cast_w("w_up", w_up_b, w_up_d, D)
        cast_w("w_down", w_down_b, w_down_d, DFF)

        wqabs = fw.sb("wqabs", [128, 2, 1024], BF16)
        wqidx = fw.sb("wqidx", [128, 2, 512], BF16)
        zzi = fw.sb("zzi", [128, 384], F32)
        convw = fw.sb("convw", [128, 48], F32)
        setup_t = []
        w_uqT = fw.sb("w_uqT", [64, 8, 256], F32); setup_t.append(w_uqT)
        w_ukd = fw.sb("w_ukd", [64, 8, 128], F32); setup_t.append(w_ukd)
        w_uvT = fw.sb("w_uvT", [64, 8, 128], F32); setup_t.append(w_uvT)
        w_outB = fw.sb("w_outB", [64, 8, 1024], F32); setup_t.append(w_outB)
        stg = fw.sb("stg", [128, 1024], BF16); setup_t.append(stg)
        fw.dma("sp", w_uqT[:].rearrange("p h k -> p (h k)"), w_uqT_d, out_t=w_uqT, shared=True)
        fw.dma("sp", w_ukd[:].rearrange("p h k -> p (h k)"), w_ukd_d, out_t=w_ukd, shared=True)
        fw.dma("sp", w_uvT[:].rearrange("p h k -> p (h k)"), w_uvT_d, out_t=w_uvT, shared=True)
        fw.dma("sp", w_outB[:].rearrange("p h k -> p (h k)"), w_outB_d, out_t=w_outB, shared=True)
        fw.freeze_shared()

        op("dve", lambda e: e.tensor_copy(idb[:], ident), r=[consts], w=[idb])
        op("dve", lambda e: e.memset(onesb[:], 1.0), w=[onesb])
        op("dve", lambda e: e.memset(cc[:, 0:1], EPS), w=[cc])
        op("dve", lambda e: e.memset(cc[:, 1:2], 1.0), w=[cc])
        op("dve", lambda e: e.memset(cc[:, 2:3], TOPK - 0.5), w=[cc])
        op("dve", lambda e: e.memset(cc[:, 3:4], 0.0), w=[cc])
        op("act", lambda e: e.activation(aneg[:], rows_s[:, R_ALOG:R_ALOG + 4], AF.Exp), r=[rows_s], w=[aneg])
        op("dve", lambda e: e.tensor_scalar(aneg[:], aneg[:], -1.0, None, ALU.mult), r=[aneg], w=[aneg])
        op("dve", lambda e: e.tensor_scalar(nrb31[:], rows_s[:, R_RB31:R_RB31 + 8], -1.0, None, ALU.mult), r=[rows_s], w=[nrb31])

        for kt in range(2):
            for hb in range(2):
                pb = pn()
                for hh in range(4):
                    h = hb * 4 + hh
                    op("pe", lambda e: e.matmul(pb[:, hh * 128:(hh + 1) * 128], w_uqT[:, h, kt * 128:(kt + 1) * 128], w_ukd[:, h, :], start=True, stop=True),
                       r=[w_uqT, w_ukd], w=[pb], signal=(hh == 3))
                op("act", lambda e: e.copy(wqabs[:, kt, hb * 512:(hb + 1) * 512], pb[:]), r=[pb], w=[wqabs])
        for h in range(8):
            for nb in range(2):
                pb = pn()
                op("pe", lambda e: e.matmul(pb[:], w_uvT[:, h, :], w_outB[:, h, nb * 512:(nb + 1) * 512], start=True, stop=True),
                   r=[w_uvT, w_outB], w=[pb])
                op("act", lambda e: e.copy(stg[:, nb * 512:(nb + 1) * 512], pb[:]), r=[pb], w=[stg])
            fw.dma("sp", w_mix_b[512 + h * 128:512 + (h + 1) * 128, :], stg[:], in_t=stg, dram_out=dmixB)
        fw.free(setup_t)
        fw.dma("sp", wqidx[:], w_qidx_b.rearrange("(k p) c -> p k c", p=128), out_t=wqidx, dram_in=deps["w_qidx"])

        def norm_A(src_ap, src_t, g_ap, g_t, tmp):
            st, junk, xn = tmp
            op("dve", lambda e: e.memset(st[:, 0:1], 0.0), w=[st])
            op("act", lambda e: e.activation(junk[:], src_ap, AF.Square, accum_out=st[:, 0:1]), r=[src_t], w=[junk, st])
            op("act", lambda e: e.activation(st[:, 1:2], st[:, 0:1], AF.Ln, bias=cc[:, 0:1], scale=1.0 / D), r=[st, cc], w=[st])
            op("act", lambda e: e.activation(st[:, 2:3], st[:, 1:2], AF.Exp, scale=-0.5), r=[st], w=[st])
            op("dve", lambda e: e.scalar_tensor_tensor(xn[:], src_ap, st[:, 2:3], g_ap, ALU.mult, ALU.mult), r=[src_t, st, g_t], w=[xn])

        def norm_B(dstT_ap3, dst_t, tmp):
            st, junk, xn = tmp
            pt = ptn()
            for k in range(8):
                op("pe", lambda e: e.transpose(pt[:, k * 128:(k + 1) * 128], xn[:, k * 128:(k + 1) * 128], idb[:]), r=[xn, idb], w=[pt], signal=(k == 7))
            op("act", lambda e: e.copy(dstT_ap3, pt[:].rearrange("p (k t) -> p k t", k=8)), r=[pt], w=[dst_t])

        def norm_T(src_ap, src_t, g_ap, g_t, dstT_ap3, dst_t, tmp):
            norm_A(src_ap, src_t, g_ap, g_t, tmp)
            norm_B(dstT_ap3, dst_t, tmp)

        CW = float((8 ** -0.5) * (64 ** -0.5))
        op("dve", lambda e: e.memset(zzi[:, 0:256], 0.0), w=[zzi])
        op("dve", lambda e: e.tensor_copy(zzi[:, 256:384], ident), r=[consts], w=[zzi])
        fw.dma("sp", convw[:], convw_d, out_t=convw)

        def T128(tt):
            return slice(tt * 128, (tt + 1) * 128)

        def body(b):
            mixdn = fw.sb("mixdn", [128, 4, S], BF16, nsub=4 * NT, side="right")
            cqnT = fw.sb("cqnT", [128, 2, S], BF16, nsub=NT, side="right")
            ckvT = fw.sb("ckvT", [128, S], BF16, nsub=NT, side="right")
            ckv1 = fw.sb("ckv1", [128, NT, 130], BF16, nsub=NT, side="right")
            kidT = fw.sb("kidT", [128, S], BF16, nsub=NT, side="right")
            sc = fw.sb("sc", [128, NT, 52], F32, nsub=NT, side="right")
            smalls = [cqnT, ckvT, ckv1, kidT, sc]
            hT = fw.sb("hT", [128, 8, S], BF16, nsub=NT)
            zs = fw.sb("zs", [128, NT, 512], BF16, nsub=NT)

            t1 = []
            gbc = fw.sb("gbc", [128, D], F32); t1.append(gbc)
            xts = [fw.sb("xt%d" % i, [128, D], F32) for i in range(4)]; t1 += xts
            tmps = []
            for i in range(4):
                tm = (fw.sb("st%d" % i, [128, 8], F32), fw.sb("junk%d" % i, [128, D], BF16), fw.sb("xn%d" % i, [128, D], BF16))
                tmps.append(tm); t1 += list(tm)
            fw.dma("sp", gbc[:], rows_d[:, R_GMIX:R_GMIX + D], out_t=gbc)
            for tt in range(NT + 2):
                if tt < NT:
                    xt = xts[tt % 4]
                    fw.dma("sp", xt[:], x_d[b, T128(tt), :], out_t=xt)
                    norm_A(xt[:], xt, gbc[:], gbc, tmps[tt % 4])
                if tt >= 2:
                    t_ = tt - 2
                    norm_B(hT[:, :, T128(t_)], hT.rg(t_), tmps[t_ % 4])

            if stop <= 1:
                raise _Stop()
            p2t = []
            for i in range(4):
                tp = (fw.sb("t4_%d" % i, [128, 4], F32), fw.sb("st2_%d" % i, [128, 8], F32), fw.sb("junkq%d" % i, [128, 256], F32),
                      fw.sb("cqn%d" % i, [128, 256], BF16), fw.sb("kxc%d" % i, [128, 64], F32), fw.sb("kid2_%d" % i, [128, 128], BF16),
                      fw.sb("ez%d" % i, [128, 512], F32))
                p2t.append(tp); t1 += list(tp)
            op("dve", lambda e: e.memset(ckv1[:, :, 128:130], 1.0), w=[ckv1])
            slZ = load_w(w_in_b, deps["w_in"], 0, 8, 1536, 512)
            slS = load_w(w_in_b, deps["w_in"], 0, 8, 2048, 464)
            def ph2_tile(tt):
                t4, st2, junkq, cqn, kxc, kid2, ez = p2t[tt % 4]
                pz, pS, pg_, pt_ = P[3 * (tt % 2)], P[3 * (tt % 2) + 1], P[3 * (tt % 2) + 2], PT[tt % 2]
                for k in range(8):
                    op("pe", lambda e: e.matmul(pz[:], hT[:, k, T128(tt)], slZ[:, k, :], start=(k == 0), stop=(k == 7)), r=[hT.rg(tt), slZ], w=[pz], signal=(k == 7))
                for k in range(8):
                    op("pe", lambda e: e.matmul(pS[:, 0:464], hT[:, k, T128(tt)], slS[:, k, 0:464], start=(k == 0), stop=(k == 7)), r=[hT.rg(tt), slS], w=[pS], signal=(k == 7))
                op("dve", lambda e: e.tensor_copy(zs[:, tt, :], pz[:]), r=[pz], w=[zs.rg(tt)])
                yield
                yield
                g_ = sc.rg(tt)
                op("act", lambda e: e.activation(sc[:, tt, 0:4], pS[:, 0:4], AF.Exp, scale=-1.0), r=[pS], w=[g_])
                yield
                op("dve", lambda e: e.tensor_scalar(sc[:, tt, 0:4], sc[:, tt, 0:4], 1.0, None, ALU.add), r=[g_], w=[g_])
                op("dve", lambda e: e.reciprocal(sc[:, tt, 0:4], sc[:, tt, 0:4]), r=[g_], w=[g_])
                yield
                op("dve", lambda e: e.tensor_tensor(t4[:], pS[:, 4:8], rows_s[:, R_DTB:R_DTB + 4], ALU.add), r=[pS, rows_s], w=[t4])
                yield
                op("act", lambda e: e.activation(t4[:], t4[:], AF.Exp), r=[t4], w=[t4])
                yield
                op("act", lambda e: e.activation(t4[:], t4[:], AF.Ln, bias=cc[:, 1:2]), r=[t4, cc], w=[t4])
                yield
                op("dve", lambda e: e.tensor_tensor(sc[:, tt, 4:8], t4[:], aneg[:], ALU.mult), r=[t4, aneg], w=[g_])
                yield
                pg = pg_
                op("pe", lambda e: e.matmul(pg[:, 0:4], tri, sc[:, tt, 4:8], start=True, stop=True), r=[consts, g_], w=[pg])
                yield
                op("pe", lambda e: e.matmul(pg[:, 4:8], ones_f, sc[:, tt, 4:8], start=True, stop=True), r=[consts, g_], w=[pg])
                yield
                op("dve", lambda e: e.tensor_copy(sc[:, tt, 8:16], pg[:, 0:8]), r=[pg], w=[g_])
                yield
                op("act", lambda e: e.activation(sc[:, tt, 16:20], sc[:, tt, 8:12], AF.Exp), r=[g_], w=[g_])
                yield
                op("dve", lambda e: e.tensor_tensor(sc[:, tt, 20:24], sc[:, tt, 12:16], sc[:, tt, 8:12], ALU.subtract), r=[g_], w=[g_])
                yield
                op("act", lambda e: e.activation(sc[:, tt, 20:24], sc[:, tt, 20:24], AF.Exp), r=[g_], w=[g_])
                yield
                op("act", lambda e: e.activation(sc[:, tt, 24:28], sc[:, tt, 12:16], AF.Exp), r=[g_], w=[g_])
                yield
                op("dve", lambda e: e.tensor_tensor(sc[:, tt, 28:32], sc[:, tt, 0:4], sc[:, tt, 16:20], ALU.mult), r=[g_], w=[g_])
                yield
                op("dve", lambda e: e.tensor_scalar(sc[:, tt, 32:36], sc[:, tt, 0:4], -1.0, None, ALU.mult), r=[g_], w=[g_])
                yield
                op("act", lambda e: e.activation(sc[:, tt, 36:44], pS[:, 456:464], AF.Abs, scale=CW), r=[pS], w=[g_])
                yield
                op("act", lambda e: e.sign(sc[:, tt, 44:52], pS[:, 456:464]), r=[pS], w=[g_])
                yield
                op("dve", lambda e: e.memset(st2[:], 0.0), w=[st2])
                yield
                op("act", lambda e: e.activation(junkq[:, 0:256], pS[:, 8:264], AF.Square, accum_out=st2[:, 0:1]), r=[pS], w=[junkq, st2])
                yield
                op("act", lambda e: e.activation(junkq[:, 0:128], pS[:, 264:392], AF.Square, accum_out=st2[:, 1:2]), r=[pS], w=[junkq, st2])
                yield
                op("act", lambda e: e.activation(st2[:, 4:5], st2[:, 0:1], AF.Ln, bias=cc[:, 0:1], scale=1.0 / 256), r=[st2, cc], w=[st2])
                yield
                op("act", lambda e: e.activation(st2[:, 5:6], st2[:, 1:2], AF.Ln, bias=cc[:, 0:1], scale=1.0 / 128), r=[st2, cc], w=[st2])
                yield
                op("act", lambda e: e.activation(st2[:, 4:6], st2[:, 4:6], AF.Exp, scale=-0.5), r=[st2], w=[st2])
                yield
                op("dve", lambda e: e.scalar_tensor_tensor(cqn[:], pS[:, 8:264], st2[:, 4:5], rows_s[:, R_QN:R_QN + 256], ALU.mult, ALU.mult), r=[pS, st2, rows_s], w=[cqn])
                yield
                op("dve", lambda e: e.scalar_tensor_tensor(ckv1[:, tt, 0:128], pS[:, 264:392], st2[:, 5:6], rows_s[:, R_KVN:R_KVN + 128], ALU.mult, ALU.mult), r=[pS, st2, rows_s], w=[ckv1.rg(tt)])
                yield
                op("dve", lambda e: e.reduce_sum(st2[:, 2:3], pS[:, 392:456], AX.X), r=[pS], w=[st2])
                yield
                op("dve", lambda e: e.tensor_scalar(st2[:, 2:3], st2[:, 2:3], 1.0 / 64, None, ALU.mult), r=[st2], w=[st2])
                yield
                op("dve", lambda e: e.tensor_scalar(kxc[:], pS[:, 392:456], st2[:, 2:3], None, ALU.subtract), r=[pS, st2], w=[kxc])
                yield
                op("act", lambda e: e.activation(junkq[:, 0:64], kxc[:], AF.Square, accum_out=st2[:, 3:4]), r=[kxc], w=[junkq, st2])
                yield
                op("act", lambda e: e.activation(st2[:, 6:7], st2[:, 3:4], AF.Ln, bias=cc[:, 0:1], scale=1.0 / 64), r=[st2, cc], w=[st2])
                yield
                op("act", lambda e: e.activation(st2[:, 6:7], st2[:, 6:7], AF.Exp, scale=-0.5), r=[st2], w=[st2])
                yield
                op("dve", lambda e: e.scalar_tensor_tensor(kxc[:], kxc[:], st2[:, 6:7], rows_s[:, R_KIG:R_KIG + 64], ALU.mult, ALU.mult), r=[kxc, st2, rows_s], w=[kxc])
                yield
                op("dve", lambda e: e.tensor_tensor(kid2[:, 0:64], kxc[:], rows_s[:, R_KIB:R_KIB + 64], ALU.add), r=[kxc, rows_s], w=[kid2])
                yield
                op("dve", lambda e: e.tensor_copy(kid2[:, 64:128], kid2[:, 0:64]), r=[kid2], w=[kid2])
                yield
                pt = pt_
                op("pe", lambda e: e.transpose(pt[:, 0:128], cqn[:, 0:128], idb[:]), r=[cqn, idb], w=[pt])
                yield
                op("pe", lambda e: e.transpose(pt[:, 128:256], cqn[:, 128:256], idb[:]), r=[cqn, idb], w=[pt])
                yield
                op("pe", lambda e: e.transpose(pt[:, 256:384], ckv1[:, tt, 0:128], idb[:]), r=[ckv1.rg(tt), idb], w=[pt])
                yield
                op("pe", lambda e: e.transpose(pt[:, 384:512], kid2[:], idb[:]), r=[kid2, idb], w=[pt])
                yield
                op("act", lambda e: e.copy(cqnT[:, 0, T128(tt)], pt[:, 0:128]), r=[pt], w=[cqnT.rg(tt)])
                yield
                op("act", lambda e: e.copy(cqnT[:, 1, T128(tt)], pt[:, 128:256]), r=[pt], w=[cqnT.rg(tt)])
                yield
                op("act", lambda e: e.copy(ckvT[:, T128(tt)], pt[:, 256:384]), r=[pt], w=[ckvT.rg(tt)])
                yield
                op("act", lambda e: e.copy(kidT[:, T128(tt)], pt[:, 384:512]), r=[pt], w=[kidT.rg(tt)])
                yield

            lanes = [iter(()), iter(())]
            nxt = [0, 1]
            alive = True
            while alive:
                alive = False
                for ln_ in range(2):
                    for _step in range(2):
                        try:
                            next(lanes[ln_])
                            alive = True
                        except StopIteration:
                            if nxt[ln_] < NT:
                                lanes[ln_] = ph2_tile(nxt[ln_])
                                nxt[ln_] += 2
                                alive = True
                            break
            for tt in range(NT):
                op("act", lambda e: e.activation(zs[:, tt, :], zs[:, tt, :], AF.Silu), r=[zs.rg(tt)], w=[zs.rg(tt)])
            if b == 0:
                dump("ckv", ckv1[:, :, 0:128], ckv1, [128, NT, 128])
                dump("sc", sc[:], sc, [128, NT, 52])
                dump("cqnT", cqnT[:], cqnT, [128, 2, S])
                dump("kidT", kidT[:], kidT, [128, S])
            if stop <= 2:
                raise _Stop()
            fw.free(t1)

            t3 = []
            def A(name, shape, dt, nsub=1):
                t = fw.sb(name, shape, dt, nsub=nsub); t3.append(t); return t
            diagw = A("diagw", [128, 12, 128], BF16)
            pre = A("pre", [128, 3, 2052], BF16)
            op("dve", lambda e: e.memset(pre[:, :, 0:4], 0.0), w=[pre])
            qtok = A("qtok", [128, NT, 128], BF16, NT)
            ktok = A("ktok", [128, NT, 128], BF16, NT)
            vtok = A("vtok", [128, NT, 128], BF16, NT)
            kT = A("kT", [128, S], BF16, NT)
            qT = A("qT", [128, S], BF16, NT)
            st3 = [A("st3_%d" % i, [128, 8], F32) for i in range(2)]
            qs4 = [A("qs4_%d" % i, [128, 384], F32) for i in range(4)]
            st4 = [A("st4_%d" % i, [128, 8], F32) for i in range(4)]
            jk4 = [A("jk4_%d" % i, [128, 128], BF16) for i in range(4)]
            GD = 4
            Gh = [A("Gh%d" % i, [128, 128], F32) for i in range(GD)]
            Ee = [A("Ee%d" % i, [128, 256], F32) for i in range(GD)]
            Es = [A("Es%d" % i, [128, 128], F32) for i in range(GD)]
            ETm = [A("ETm%d" % i, [128, 128], BF16) for i in range(GD)]
            qkTm = [[A("qkTm%d_%d" % (g, i), [128, 128], BF16) for i in range(GD)] for g in range(2)]
            M0 = [A("M0_%d" % i, [128, 128], BF16) for i in range(GD)]
            Mx = [[A("Mx%d_%d" % (i, k), [128, 3, 128], BF16) for k in range(2)] for i in range(GD)]
            Xb = [[A("Xb%d_%d" % (i, k), [128, 256], BF16) for k in range(2)] for i in range(GD)]
            u32 = [[A("u32_%d_%d" % (g, i), [128, 128], F32) for i in range(GD)] for g in range(2)]
            wb = [A("wb%d" % i, [128, 128], BF16) for i in range(GD)]
            qdec = [A("qdec%d" % i, [128, 128], BF16) for i in range(GD)]
            wq = [[A("wq%d_%d" % (g, i), [128, 256], BF16) for i in range(GD)] for g in range(2)]
            kdec = [[A("kdec%d_%d" % (g, i), [128, 128], BF16) for i in range(GD)] for g in range(2)]
            vnew = [A("vnew%d" % i, [128, 128], BF16) for i in range(2)]
            ot = [A("ot%d" % i, [128, 128], F32) for i in range(2)]
            on = [A("on%d" % i, [128, 128], BF16) for i in range(2)]
            jk3 = [A("jk3_%d" % i, [128, 128], BF16) for i in range(2)]
            Sf = A("Sf", [128, 128], F32)
            Sb = A("Sb", [128, 128], BF16)

            for h in range(4):
                sctr[0] = (sctr[0] + 1) % len(slots)
                sl = slots[sctr[0]]
                for i in range(12):
                    c3_, j_ = i // 4, i % 4
                    ci = (c3_ * 4 + h) * 4 + j_
                    op("dve" if i % 2 else "pool", lambda e: e.tensor_scalar(diagw[:, i, :], ident, convw[:, ci:ci + 1], None, ALU.mult), r=[consts, convw], w=[diagw])
                for c3 in range(3):
                    c0 = c3 * 512 + h * 128
                    fw.dma("sp", sl[:, :, c3 * 128:(c3 + 1) * 128], w_in_b[:, c0:c0 + 128].rearrange("(k p) c -> p k c", p=128),
                           out_t=sl, dram_in=deps["w_in"])
                for c3 in range(3):
                    for tb in range(4):
                        pb = pn()
                        for k in range(8):
                            op("pe", lambda e: e.matmul(pb[:], sl[:, k, c3 * 128:(c3 + 1) * 128], hT[:, k, tb * 512:(tb + 1) * 512], start=(k == 0), stop=(k == 7)),
                               r=[sl, hT.rg(tb * 4, tb * 4 + 4)], w=[pb], signal=(k == 7))
                        if tb % 2:
                            op("act", lambda e: e.copy(pre[:, c3, 4 + tb * 512:4 + (tb + 1) * 512], pb[:]), r=[pb], w=[pre])
                        else:
                            op("dve", lambda e: e.tensor_copy(pre[:, c3, 4 + tb * 512:4 + (tb + 1) * 512], pb[:]), r=[pb], w=[pre])
                if CUT == 41:
                    raise _Stop()
                def conv_a1(tt):
                    pb = pn()
                    for c3 in range(3):
                        for j in range(4):
                            op("pe", lambda e: e.matmul(pb[:, c3 * 128:(c3 + 1) * 128], pre[:, c3, tt * 128 + 1 + j:tt * 128 + 1 + j + 128], diagw[:, c3 * 4 + j, :], start=(j == 0), stop=(j == 3)),
                               r=[pre, diagw], w=[pb], signal=(c3 == 2 and j == 3))
                    q_ = qs4[tt % 4]
                    op("act", lambda e: e.activation(q_[:], pb[:, 0:384], AF.Silu), r=[pb], w=[q_])

                def conv_a2(tt):
                    q_ = qs4[tt % 4]; s_ = st4[tt % 4]; jk = jk4[tt % 4]
                    op("dve", lambda e: e.memset(s_[:], 0.0), w=[s_])
                    op("act", lambda e: e.activation(jk[:], q_[:, 0:128], AF.Square, accum_out=s_[:, 0:1]), r=[q_], w=[jk, s_])
                    op("act", lambda e: e.activation(jk[:], q_[:, 128:256], AF.Square, accum_out=s_[:, 1:2]), r=[q_], w=[jk, s_])
                    op("act", lambda e: e.activation(s_[:, 2:4], s_[:, 0:2], AF.Ln, bias=cc[:, 0:1]), r=[s_, cc], w=[s_])
                    op("act", lambda e: e.activation(s_[:, 2:4], s_[:, 2:4], AF.Exp, scale=-0.5), r=[s_], w=[s_])
                    op("dve", lambda e: e.tensor_scalar(qtok[:, tt, :], q_[:, 0:128], s_[:, 2:3], float(128 ** -0.5), ALU.mult, ALU.mult), r=[q_, s_], w=[qtok.rg(tt)])
                    op("dve", lambda e: e.tensor_scalar(ktok[:, tt, :], q_[:, 128:256], s_[:, 3:4], None, ALU.mult), r=[q_, s_], w=[ktok.rg(tt)])
                    op("pool", lambda e: e.tensor_copy(vtok[:, tt, :], q_[:, 256:384]), r=[q_], w=[vtok.rg(tt)])
                def conv_b(tt):
                    pt = ptn()
                    op("pe", lambda e: e.transpose(pt[:, 0:128], ktok[:, tt, :], idb[:]), r=[ktok.rg(tt), idb], w=[pt], signal=False)
                    op("pe", lambda e: e.transpose(pt[:, 128:256], qtok[:, tt, :], idb[:]), r=[qtok.rg(tt), idb], w=[pt])
                    op("act", lambda e: e.copy(kT[:, T128(tt)], pt[:, 0:128]), r=[pt], w=[kT.rg(tt)])
                    op("act", lambda e: e.copy(qT[:, T128(tt)], pt[:, 128:256]), r=[pt], w=[qT.rg(tt)])
                for tb4 in range(0, NT, 4):
                    for tt in range(tb4, tb4 + 4):
                        conv_a1(tt)
                    for tt in range(tb4, tb4 + 4):
                        conv_a2(tt)
                    for tt in range(tb4, tb4 + 4):
                        conv_b(tt)
                if b == 0 and h == 0:
                    dump("qtok", qtok[:], qtok, [128, NT, 128])
                    dump("ktok", ktok[:], ktok, [128, NT, 128])
                    dump("vtok", vtok[:], vtok, [128, NT, 128])
                if CUT == 42:
                    raise _Stop()
                op("dve", lambda e: e.memset(Sf[:], 0.0), w=[Sf])
                op("dve", lambda e: e.memset(Sb[:], 0.0), w=[Sb])
                def dn_prep(cg, gp):
                    cs = list(range(cg, cg + GD))
                    colf = lambda c, o: sc[:, c, o + h:o + h + 1]
                    pDs = []
                    for j, c in enumerate(cs):
                        op("act", lambda e: e.mul(Gh[j][:], su, colf(c, 4)), r=[consts, sc.rg(c)], w=[Gh[j]])
                    yield
                    for j, c in enumerate(cs):
                        pD = pn(0, 4); pDs.append(pD)
                        op("pe", lambda e: e.matmul(pD[:, 0:128], tri, Gh[j][:], start=True, stop=True), r=[consts, Gh[j]], w=[pD], signal=False)
                        op("pe", lambda e: e.matmul(pD[:, 128:256], Gh[j][:], tri, start=True, stop=True), r=[consts, Gh[j]], w=[pD])
                    yield
                    for j, c in enumerate(cs):
                        op("act", lambda e: e.activation(Ee[j][:], pDs[j][:, 0:256], AF.Exp), r=[pDs[j]], w=[Ee[j]])
                    for j, c in enumerate(cs):
                        op("pool", lambda e: e.tensor_tensor(Es[j][:], Ee[j][:, 0:128], slm, ALU.mult), r=[Ee[j], consts], w=[Es[j]])
                        op("pool", lambda e: e.tensor_tensor(ETm[j][:], Ee[j][:, 128:256], ium, ALU.mult), r=[Ee[j], consts], w=[ETm[j]])
                    yield
                    pKs = []
                    for j, c in enumerate(cs):
                        pK = pn(0, 4); pKs.append(pK)
                        op("pe", lambda e: e.matmul(pK[:, 0:128], kT[:, T128(c)], kT[:, T128(c)], start=True, stop=True), r=[kT.rg(c)], w=[pK], signal=False)
                        op("pe", lambda e: e.matmul(pK[:, 128:256], kT[:, T128(c)], qT[:, T128(c)], start=True, stop=True), r=[kT.rg(c), qT.rg(c)], w=[pK])
                    for j, c in enumerate(cs):
                        op("dve", lambda e: e.scalar_tensor_tensor(M0[j][:], pKs[j][:, 0:128], colf(c, 32), Es[j][:], ALU.mult, ALU.mult), r=[pKs[j], sc.rg(c), Es[j]], w=[M0[j]])
                        op("dve", lambda e: e.tensor_tensor(qkTm[gp][j][:], pKs[j][:, 128:256], ETm[j][:], ALU.mult), r=[pKs[j], ETm[j]], w=[qkTm[gp][j]])
                    yield
                    pt = ptn()
                    for j, c in enumerate(cs):
                        op("pe", lambda e: e.transpose(pt[:, j * 128:(j + 1) * 128], M0[j][:], idb[:]), r=[M0[j], idb], w=[pt], signal=(j == GD - 1))
                    for j, c in enumerate(cs):
                        cur = Mx[j][0]
                        op("dve", lambda e: e.tensor_copy(cur[:, 0, :], M0[j][:]), r=[M0[j]], w=[cur])
                        op("act", lambda e: e.copy(cur[:, 1, :], pt[:, j * 128:(j + 1) * 128]), r=[pt], w=[cur])
                        op("pool", lambda e: e.tensor_tensor(cur[:, 2, :], cur[:, 1, :], idb[:], ALU.add), r=[cur, idb], w=[cur])
                        X = Xb[j][0]
                        op("dve", lambda e: e.tensor_scalar(X[:, 0:128], vtok[:, c, :], colf(c, 0), None, ALU.mult), r=[vtok.rg(c), sc.rg(c)], w=[X])
                        op("dve", lambda e: e.tensor_scalar(X[:, 128:256], ktok[:, c, :], colf(c, 28), None, ALU.mult), r=[ktok.rg(c), sc.rg(c)], w=[X])
                    yield
                    for l in range(7):
                        yield
                        pXs = []
                        for j, c in enumerate(cs):
                            cur = Mx[j][l % 2]
                            pX = pn(0, 4); pXs.append(pX)
                            op("pe", lambda e: e.matmul(pX[:, 0:256], cur[:, 2, :], Xb[j][l % 2][:], start=True, stop=True), r=[cur, Xb[j][l % 2]], w=[pX])
                        yield
                        for j, c in enumerate(cs):
                            if l < 6:
                                op("act", lambda e: e.copy(Xb[j][(l + 1) % 2][:], pXs[j][:, 0:256]), r=[pXs[j]], w=[Xb[j][(l + 1) % 2]])
                            else:
                                op("act", lambda e: e.copy(u32[gp][j][:], pXs[j][:, 0:128]), r=[pXs[j]], w=[u32[gp][j]])
                                op("act", lambda e: e.copy(wb[j][:], pXs[j][:, 128:256]), r=[pXs[j]], w=[wb[j]])
                        yield
                        if l < 6:
                            pMs = []
                            for j, c in enumerate(cs):
                                cur = Mx[j][l % 2]
                                pM = pn(0, 4); pMs.append(pM)
                                if l < 5:
                                    op("pe", lambda e: e.matmul(pM[:, 0:128], cur[:, 1, :], cur[:, 0, :], start=True, stop=True), r=[cur], w=[pM], signal=False)
                                    op("pe", lambda e: e.matmul(pM[:, 128:256], cur[:, 0, :], cur[:, 1, :], start=True, stop=True), r=[cur], w=[pM], signal=False)
                                    op("pe", lambda e: e.matmul(pM[:, 256:384], cur[:, 0, :], cur[:, 1, :], start=True, stop=True), r=[cur], w=[pM])
                                else:
                                    op("pe", lambda e: e.matmul(pM[:, 0:128], cur[:, 0, :], cur[:, 1, :], start=True, stop=True), r=[cur], w=[pM])
                            for j, c in enumerate(cs):
                                nxt = Mx[j][(l + 1) % 2]
                                if l < 5:
                                    op("dve", lambda e: e.tensor_tensor(nxt[:].rearrange("p a b -> p (a b)"), pMs[j][:, 0:384], zzi[:], ALU.add), r=[pMs[j], zzi], w=[nxt])
                                else:
                                    op("dve", lambda e: e.tensor_tensor(nxt[:, 2, :], pMs[j][:, 0:128], ident, ALU.add), r=[pMs[j], consts], w=[nxt])
                    yield
                    for j, c in enumerate(cs):
                        op("act", lambda e: e.mul(qdec[j][:], qtok[:, c, :], colf(c, 16)), r=[qtok.rg(c), sc.rg(c)], w=[qdec[j]])
                        op("act", lambda e: e.mul(kdec[gp][j][:], ktok[:, c, :], colf(c, 20)), r=[ktok.rg(c), sc.rg(c)], w=[kdec[gp][j]])
                    pt = ptn()
                    for j, c in enumerate(cs):
                        op("pe", lambda e: e.transpose(pt[:, j * 256:j * 256 + 128], wb[j][:], idb[:]), r=[wb[j], idb], w=[pt], signal=False)
                        op("pe", lambda e: e.transpose(pt[:, j * 256 + 128:(j + 1) * 256], qdec[j][:], idb[:]), r=[qdec[j], idb], w=[pt], signal=(j == GD - 1))
                    for j, c in enumerate(cs):
                        op("act", lambda e: e.copy(wq[gp][j][:], pt[:, j * 256:(j + 1) * 256]), r=[pt], w=[wq[gp][j]])
                    yield
                def dn_recur(cg, gp, gen):
                    cs = list(range(cg, cg + GD))
                    colf = lambda c, o: sc[:, c, o + h:o + h + 1]
                    def pull(n):
                        if gen is not None:
                            for _ in range(n):
                                next(gen, None)
                    for j, c in enumerate(cs):
                        i2 = c % 2
                        g_ = sc.rg(c)
                        p1 = P[4]
                        op("pe", lambda e: e.matmul(p1[:, 0:128], wq[gp][j][:, 0:128], Sb[:], start=True, stop=True), r=[wq[gp][j], Sb], w=[p1])
                        pull(2)
                        op("dve", lambda e: e.tensor_tensor(vnew[i2][:], u32[gp][j][:], p1[:, 0:128], ALU.subtract), r=[u32[gp][j], p1], w=[vnew[i2]])
                        pull(2)
                        p2 = P[5]
                        op("pe", lambda e: e.matmul(p2[:, 0:128], wq[gp][j][:, 128:256], Sb[:], start=True, stop=False), r=[wq[gp][j], Sb], w=[p2], signal=False)
                        op("pe", lambda e: e.matmul(p2[:, 0:128], qkTm[gp][j][:], vnew[i2][:], start=False, stop=True), r=[qkTm[gp][j], vnew[i2]], w=[p2], signal=False)
                        op("pe", lambda e: e.matmul(p2[:, 128:256], kdec[gp][j][:], vnew[i2][:], start=True, stop=True), r=[kdec[gp][j], vnew[i2]], w=[p2])
                        pull(2)
                        op("dve", lambda e: e.scalar_tensor_tensor(Sf[:], Sf[:], colf(c, 24), p2[:, 128:256], ALU.mult, ALU.add), r=[Sf, g_, p2], w=[Sf])
                        op("act", lambda e: e.copy(Sb[:], Sf[:]), r=[Sf], w=[Sb])
                        pull(2)
                        s_ = st3[i2]
                        op("dve", lambda e: e.memset(s_[:], 0.0), w=[s_])
                        op("act", lambda e: e.activation(jk3[i2][:], p2[:, 0:128], AF.Square, accum_out=s_[:, 0:1]), r=[p2], w=[jk3[i2], s_])
                        op("act", lambda e: e.activation(s_[:, 1:2], s_[:, 0:1], AF.Ln, bias=cc[:, 0:1], scale=1.0 / 128), r=[s_, cc], w=[s_])
                        op("act", lambda e: e.activation(s_[:, 1:2], s_[:, 1:2], AF.Exp, scale=-0.5), r=[s_], w=[s_])
                        op("dve", lambda e: e.scalar_tensor_tensor(ot[i2][:], p2[:, 0:128], s_[:, 1:2], rows_s[:, R_DNG:R_DNG + 128], ALU.mult, ALU.mult), r=[p2, s_, rows_s], w=[ot[i2]])
                        op("pool", lambda e: e.tensor_tensor(on[i2][:], ot[i2][:], zs[:, c, h * 128:(h + 1) * 128], ALU.mult), r=[ot[i2], zs.rg(c)], w=[on[i2]])
                        pt2 = ptn()
                        op("pe", lambda e: e.transpose(pt2[:, 0:128], on[i2][:], idb[:]), r=[on[i2], idb], w=[pt2])
                        op("act", lambda e: e.copy(mixdn[:, h, T128(c)], pt2[:, 0:128]), r=[pt2], w=[mixdn.rg(h * NT + c)])
                        pull(2)
                    if gen is not None:
                        for _ in gen:
                            pass
                for _ in dn_prep(0, 0):
                    pass
                ngrp = NT // GD
                for gi in range(ngrp):
                    gen = dn_prep((gi + 1) * GD, (gi + 1) % 2) if gi + 1 < ngrp else None
                    dn_recur(gi * GD, gi % 2, gen)
            if b == 0:
                dump("mixdn", mixdn[:], mixdn, [128, 4, S])
            if stop <= 3:
                raise _Stop()
            fw.free(t3)
            fw.free([hT, zs])
            mixdsa = fw.sb("mixdsa", [128, 8, S], BF16, nsub=8 * (NT // GQ))
            t4_ = []
            def A(name, shape, dt, nsub=1):
                t = fw.sb(name, shape, dt, nsub=nsub); t4_.append(t); return t
            Eb = A("Eb", [128, 8, 256], BF16)
            qabsT = A("qabsT", [128, 8, 512], BF16, 8)
            qidxT2 = [A("qidxT%d" % i, [128, 4, 512], BF16, 4) for i in range(2)]
            score = [A("score%d" % i, [128, S], F32) for i in range(GQ)]
            btf = score[0]
            fw.dma("sp", btf[:], bt_d, out_t=btf)
            for h in range(8):
                op("act", lambda e: e.activation(Eb[:, h, :], btf[:, h * 256:(h + 1) * 256], AF.Exp, bias=nrb31[:, h:h + 1]), r=[btf, nrb31], w=[Eb])
            tmpS = [A("tmpS%d" % i, [128, 512], F32) for i in range(3)]
            maskb = [A("maskb%d" % i, [128, S], BF16) for i in range(2)]
            junkS = maskb[0]
            maskT2 = [A("maskT%d" % i, [128, NT, 512], BF16, GQ) for i in range(2)]
            Pm = [A("Pm%d" % i, [128, 512], BF16) for i in range(4)]
            bs2 = [A("bs%d" % i, [128, 40], F32) for i in range(2)]
            rd = [A("rd%d" % i, [128, 4], F32) for i in range(2)]
            otok = [A("otok%d" % i, [128, 512], BF16) for i in range(2)]
            HI, LO, W0, MID, CNT, TA, THR = 0, 4, 8, 12, 16, 20, 24
            ictr2 = [0]
            pmctr = [0]

            def dsa_prep(gq):
                q0 = gq * GQ * 128
                qidxT = qidxT2[gq % 2]; maskT = maskT2[gq % 2]; bs = bs2[gq % 2]
                for pr in range(4):
                    pb = pn(0, 4)
                    for k in range(2):
                        op("pe", lambda e: e.matmul(pb[:], wqidx[:, k, pr * 128:(pr + 1) * 128], cqnT[:, k, q0:q0 + 512], start=(k == 0), stop=(k == 1)),
                           r=[wqidx, cqnT.rg(gq * GQ, gq * GQ + GQ)], w=[pb], signal=(k == 1))
                    op("act", lambda e: e.copy(qidxT[:, pr, :], pb[:]), r=[pb], w=[qidxT.rg(pr)])
                yield
                for ql in range(GQ):
                    qt = gq * GQ + ql
                    NK = (qt + 1) * 128
                    scr = score[ql]
                    for h in range(8):
                        pr, lo = h // 2, (h % 2) * 64
                        for sb0 in range(0, NK, 512):
                            n = min(512, NK - sb0)
                            pb = pn(0, 4)
                            op("pe", lambda e: e.matmul(pb[:, 0:n], qidxT[lo:lo + 64, pr, ql * 128:(ql + 1) * 128], kidT[lo:lo + 64, sb0:sb0 + n], start=True, stop=True),
                               r=[qidxT.rg(pr), kidT.rg(sb0 // 128, (sb0 + n) // 128)], w=[pb])
                            ictr2[0] += 1
                            tm = tmpS[ictr2[0] % 3]
                            op("act", lambda e: e.activation(tm[:, 0:n], pb[:, 0:n], AF.Relu, scale=sc[:, qt, 36 + h:37 + h]), r=[pb, sc.rg(qt)], w=[tm])
                            if h == 0:
                                op("dve", lambda e: e.tensor_scalar(scr[:, sb0:sb0 + n], tm[:, 0:n], sc[:, qt, 44 + h:45 + h], None, ALU.mult), r=[tm, sc.rg(qt)], w=[scr])
                            else:
                                op("dve", lambda e: e.scalar_tensor_tensor(scr[:, sb0:sb0 + n], tm[:, 0:n], sc[:, qt, 44 + h:45 + h], scr[:, sb0:sb0 + n], ALU.mult, ALU.add), r=[tm, sc.rg(qt), scr], w=[scr])
                        yield
                act_q = [ql for ql in range(GQ) if gq * GQ + ql >= 2]
                for ql in act_q:
                    NK = (gq * GQ + ql + 1) * 128
                    scr = score[ql]
                    op("dve", lambda e: e.reduce_max(bs[:, HI + ql:HI + ql + 1], scr[:, 0:NK], AX.X), r=[scr], w=[bs])
                    op("dve", lambda e: e.tensor_reduce(bs[:, LO + ql:LO + ql + 1], scr[:, 0:NK], AX.X, ALU.min), r=[scr], w=[bs])
                    op("pool", lambda e: e.tensor_tensor(scr[:, NK - 128:NK], scr[:, NK - 128:NK], cmm, ALU.add), r=[scr, consts], w=[scr])
                    yield
                if act_q:
                    a0, a1 = act_q[0], act_q[-1] + 1
                    cs = lambda o: bs[:, o + a0:o + a1]
                    op("dve", lambda e: e.tensor_tensor(cs(W0), cs(HI), cs(LO), ALU.subtract), r=[bs], w=[bs])
                    op("dve", lambda e: e.scalar_tensor_tensor(cs(MID), cs(W0), 0.5, cs(LO), ALU.mult, ALU.add), r=[bs], w=[bs])
                    for it in range(NBIS):
                        op("dve", lambda e: e.memset(cs(CNT), 0.0), w=[bs])
                        for ql in act_q:
                            NK = (gq * GQ + ql + 1) * 128
                            scr = score[ql]
                            op("dve", lambda e: e.tensor_scalar(junkS[:, 0:NK], scr[:, 0:NK], bs[:, MID + ql:MID + ql + 1], 0.0, ALU.is_ge, ALU.add, accum_out=bs[:, CNT + ql:CNT + ql + 1]),
                               r=[scr, bs], w=[junkS, bs])
                        if it < NBIS - 1:
                            f = 2.0 ** -(it + 2)
                            op("dve", lambda e: e.tensor_scalar(cs(TA), cs(CNT), TOPK - 0.5, None, ALU.is_ge), r=[bs], w=[bs])
                            op("dve", lambda e: e.tensor_scalar(cs(TA), cs(TA), 2.0 * f, -f, ALU.mult, ALU.add), r=[bs], w=[bs])
                            op("dve", lambda e: e.tensor_tensor(cs(TA), cs(TA), cs(W0), ALU.mult), r=[bs], w=[bs])
                            op("dve", lambda e: e.tensor_tensor(cs(MID), cs(MID), cs(TA), ALU.add), r=[bs], w=[bs])
                        else:
                            f = 2.0 ** -(it + 1)
                            op("dve", lambda e: e.tensor_scalar(cs(TA), cs(CNT), TOPK - 0.5, -f, ALU.is_lt, ALU.mult), r=[bs], w=[bs])
                            op("dve", lambda e: e.tensor_tensor(cs(TA), cs(TA), cs(W0), ALU.mult), r=[bs], w=[bs])
                            op("dve", lambda e: e.tensor_tensor(cs(THR), cs(MID), cs(TA), ALU.add), r=[bs], w=[bs])
                        yield
                mreg = maskT.regs
                for ql in range(GQ):
                    qt = gq * GQ + ql
                    NK = (qt + 1) * 128
                    mb = maskb[ql % 2]
                    if qt >= 2:
                        op("dve", lambda e: e.tensor_scalar(mb[:, 0:NK], score[ql][:, 0:NK], bs[:, THR + ql:THR + ql + 1], None, ALU.is_ge), r=[score[ql], bs], w=[mb])
                    else:
                        if NK > 128:
                            op("dve", lambda e: e.memset(mb[:, 0:NK - 128], 1.0), w=[mb])
                        op("dve", lambda e: e.tensor_copy(mb[:, NK - 128:NK], lim), r=[consts], w=[mb])
                    for k0 in range(0, qt + 1, 8):
                        n8 = min(8, qt + 1 - k0)
                        pt = ptn()
                        for i in range(n8):
                            op("pe", lambda e: e.transpose(pt[:, i * 128:(i + 1) * 128], mb[:, (k0 + i) * 128:(k0 + i + 1) * 128], idb[:]), r=[mb, idb], w=[pt], signal=(i == n8 - 1))
                        op("act", lambda e: e.copy(maskT[:, k0:k0 + n8, ql * 128:(ql + 1) * 128], pt[:, 0:n8 * 128].rearrange("p (k t) -> p k t", k=n8)), r=[pt], w=[mreg[ql]])
                    yield

            def dsa_attn(gq, gen):
                q0 = gq * GQ * 128
                maskT = maskT2[gq % 2]
                for h in range(8):
                    pb = pn(0, 4)
                    for k in range(2):
                        op("pe", lambda e: e.matmul(pb[:], wqabs[:, k, h * 128:(h + 1) * 128], cqnT[:, k, q0:q0 + 512], start=(k == 0), stop=(k == 1)),
                           r=[wqabs, cqnT.rg(gq * GQ, gq * GQ + GQ)], w=[pb], signal=(k == 1))
                    op("act", lambda e: e.copy(qabsT[:, h, :], pb[:]), r=[pb], w=[qabsT.rg(h)])
                nkt = (gq + 1) * GQ
                npull = max(1, -(-64 // (8 * nkt))) if gen is not None else 0
                acc = [P[4], P[5]]

                def stage_a(h, kt):
                    ql0 = max(kt - gq * GQ, 0)
                    c0 = ql0 * 128
                    ncol = 512 - c0
                    pL = pn(0, 4)
                    op("pe", lambda e: e.matmul(pL[:, 0:ncol], ckvT[:, T128(kt)], qabsT[:, h, c0:512], start=True, stop=True), r=[ckvT.rg(kt), qabsT.rg(h)], w=[pL])
                    pmctr[0] += 1
                    pm = Pm[pmctr[0] % 4]
                    op("act", lambda e: e.activation(pm[:, 0:ncol], pL[:, 0:ncol], AF.Exp, scale=0.125), r=[pL], w=[pm])
                    op("dve", lambda e: e.tensor_tensor(pm[:, 0:ncol], pm[:, 0:ncol], maskT[:, kt, c0:512], ALU.mult), r=[pm, maskT], w=[pm])
                    bst = (kt - gq * GQ) * 128
                    lo_c, hi_c = max(bst, 0), min(bst + 256, 512)
                    if hi_c > lo_c:
                        op("dve", lambda e: e.tensor_tensor(pm[:, lo_c - c0:hi_c - c0], pm[:, lo_c - c0:hi_c - c0], Eb[:, h, lo_c - bst:hi_c - bst], ALU.mult), r=[pm, Eb], w=[pm])
                    return pm

                def stage_b(h, kt, pm):
                    ql0 = max(kt - gq * GQ, 0)
                    for ql in range(ql0, GQ):
                        qt = gq * GQ + ql
                        a = acc[ql // 2]
                        off = (ql % 2) * 130
                        op("pe", lambda e: e.matmul(a[:, off:off + 129], pm[:, (ql - ql0) * 128:(ql - ql0 + 1) * 128], ckv1[:, kt, 0:129], start=(kt == 0 and ql % 2 == 0), stop=(kt == qt), skip_group_check=True),
                           r=[pm, ckv1.rg(kt)], w=[a], signal=(ql == GQ - 1))

                def finalize(h):
                    r_ = rd[h % 2]
                    o_ = otok[h % 2]
                    pt = ptn()
                    for ql in range(GQ):
                        a = acc[ql // 2]
                        off = (ql % 2) * 130
                        op("dve", lambda e: e.reciprocal(r_[:, ql:ql + 1], a[:, off + 128:off + 129]), r=[a], w=[r_])
                        op("dve", lambda e: e.tensor_scalar(o_[:, ql * 128:(ql + 1) * 128], a[:, off:off + 128], r_[:, ql:ql + 1], None, ALU.mult), r=[a, r_], w=[o_])
                    for ql in range(GQ):
                        op("pe", lambda e: e.transpose(pt[:, ql * 128:(ql + 1) * 128], o_[:, ql * 128:(ql + 1) * 128], idb[:]), r=[o_, idb], w=[pt], signal=(ql == GQ - 1))
                    op("act", lambda e: e.copy(mixdsa[:, h, q0:q0 + 512], pt[:, 0:512]), r=[pt], w=[mixdsa.rg(h * (NT // GQ) + gq)])

                items = [(h, kt) for h in range(8) for kt in range(nkt)]
                LAG = 2
                pend_pm = {}
                for idx in range(len(items) + LAG):
                    if idx < len(items):
                        pend_pm[idx] = stage_a(*items[idx])
                    j = idx - LAG
                    if j >= 0:
                        h, kt = items[j]
                        stage_b(h, kt, pend_pm.pop(j))
                        for _ in range(npull):
                            next(gen, None)
                        if kt == nkt - 1:
                            finalize(h)
                if gen is not None:
                    for _ in gen:
                        pass

            for _ in dsa_prep(0):
                pass
            for gq in range(NT // GQ):
                gen = dsa_prep(gq + 1) if gq + 1 < NT // GQ else None
                dsa_attn(gq, gen)
            if b == 0:
                dump("mixdsa", mixdsa[:], mixdsa, [128, 8, S])
            if stop <= 4:
                raise _Stop()
            fw.free(t4_)
            fw.free(smalls)

            t5 = []
            def A(name, shape, dt, nsub=1):
                t = fw.sb(name, shape, dt, nsub=nsub); t5.append(t); return t
            gx = A("gx", [128, D], F32); gf = A("gf", [128, D], F32); gfin = A("gfin", [128, D], F32); gmem = A("gmem", [128, D], F32)
            fw.dma("sp", gx[:], rows_d[:, R_GX:R_GX + D], out_t=gx)
            fw.dma("sp", gf[:], rows_d[:, R_GF:R_GF + D], out_t=gf)
            fw.dma("sp", gfin[:], rows_d[:, R_GFIN:R_GFIN + D], out_t=gfin)
            fw.dma("sp", gmem[:], rows_d[:, R_GMEM:R_GMEM + D], out_t=gmem)
            kxT = A("kxT", [128, 8, 256], BF16)
            vx = A("vx", [128, 2, D], BF16)
            mnT = A("mnT", [128, 8, 256], BF16, 2)
            x1 = A("x1", [128, 4, D], F32, 4)
            xT = A("xT", [128, 8, 512], BF16, 4)
            tmps = []
            for i in range(4):
                tm = (A("st%d" % i, [128, 8], F32), A("junk%d" % i, [128, D], BF16), A("xn%d" % i, [128, D], BF16))
                tmps.append(tm)
            outb = [A("outb%d" % i, [128, D], F32) for i in range(2)]
            big = A("big", [128, 22, 512], BF16, 22)
            slots.append(A("slotx0", [128, 8, 512], BF16)); slots.append(A("slotx1", [128, 8, 512], BF16))
            pexp = [A("pexp%d" % i, [128, 512], BF16) for i in range(2)]
            rden = A("rden", [128, 512], F32)
            sg = [A("sg%d" % i, [128, 512], F32) for i in range(2)]
            for mt in range(2):
                xt = outb[mt]
                fw.dma("sp", xt[:], mem_d[b, mt * 128:(mt + 1) * 128, :], out_t=xt)
                norm_T(xt[:], xt, gmem[:], gmem, mnT[:, :, mt * 128:(mt + 1) * 128], mnT.rg(mt), tmps[mt])
            for nb in range(2):
                sl = load_w(w_xkv_b, deps["w_xkv"], 0, 8, nb * 512, 512)
                for ctl in range(4):
                    pb = pn()
                    for k in range(8):
                        op("pe", lambda e: e.matmul(pb[:, 0:256], sl[:, k, ctl * 128:(ctl + 1) * 128], mnT[:, k, :], start=(k == 0), stop=(k == 7)), r=[sl, mnT], w=[pb], signal=(k == 7))
                    op("act", lambda e: e.copy(kxT[:, nb * 4 + ctl, :], pb[:, 0:256]), r=[pb], w=[kxT])
            for nb in range(2):
                sl = load_w(w_xkv_b, deps["w_xkv"], 0, 8, 1024 + nb * 512, 512)
                for mt in range(2):
                    pb = pn()
                    for k in range(8):
                        op("pe", lambda e: e.matmul(pb[:], mnT[:, k, mt * 128:(mt + 1) * 128], sl[:, k, :], start=(k == 0), stop=(k == 7)), r=[sl, mnT], w=[pb], signal=(k == 7))
                    op("dve", lambda e: e.tensor_copy(vx[:, mt, nb * 512:(nb + 1) * 512], pb[:]), r=[pb], w=[vx])

            for tb in range(4):
                for tl in range(4):
                    tt = tb * 4 + tl
                    fw.dma("sp", x1[:, tl, :], x_d[b, T128(tt), :], out_t=x1.rg(tl))
                for nb in range(2):
                    slA = load_w(w_mix_b, [dmix, dmixB], 0, 8, nb * 512, 512)
                    slB = load_w(w_mix_b, [dmix, dmixB], 8, 4, nb * 512, 512)
                    for tl in range(4):
                        tt = tb * 4 + tl
                        pb = pn()
                        for k in range(12):
                            if k < 4:
                                src, sr = mixdn[:, k, T128(tt)], mixdn.rg(k * NT + tt)
                            else:
                                src, sr = mixdsa[:, k - 4, T128(tt)], mixdsa.rg((k - 4) * (NT // GQ) + tt // GQ)
                            wsl = slA if k < 8 else slB
                            op("pe", lambda e: e.matmul(pb[:], src, wsl[:, k % 8, :], start=(k == 0), stop=(k == 11)), r=[sr, wsl], w=[pb], signal=(k == 11))
                        op("dve", lambda e: e.tensor_tensor(x1[:, tl, nb * 512:(nb + 1) * 512], x1[:, tl, nb * 512:(nb + 1) * 512], pb[:], ALU.add), r=[x1.rg(tl), pb], w=[x1.rg(tl)])
                if b == 0 and tb == 0:
                    for tl in range(4):
                        dump("x1_%d" % tl, x1[:, tl, :], x1.rg(tl), [128, D])
                for tl in range(4):
                    norm_A(x1[:, tl, :], x1.rg(tl), gx[:], gx, tmps[tl])
                for tl in range(4):
                    norm_B(xT[:, :, T128(tl)], xT.rg(tl), tmps[tl])
                for nb in range(2):
                    sl = load_w(w_xq_b, deps["w_xq"], 0, 8, nb * 512, 512)
                    for ctl in range(4):
                        n8 = nb * 4 + ctl
                        pb = pn()
                        for k in range(8):
                            op("pe", lambda e: e.matmul(pb[:], sl[:, k, ctl * 128:(ctl + 1) * 128], xT[:, k, :], start=(k == 0), stop=(k == 7)), r=[sl, xT], w=[pb], signal=(k == 7))
                        if ctl % 2:
                            op("act", lambda e: e.copy(big[:, n8, :], pb[:]), r=[pb], w=[big.rg(n8)])
                        else:
                            op("dve", lambda e: e.tensor_copy(big[:, n8, :], pb[:]), r=[pb], w=[big.rg(n8)])
                for h in range(4):
                    for mt in range(2):
                        pL = pn()
                        for d2 in range(2):
                            op("pe", lambda e: e.matmul(pL[:], kxT[:, h * 2 + d2, mt * 128:(mt + 1) * 128], big[:, h * 2 + d2, :], start=(d2 == 0), stop=(d2 == 1)), r=[kxT, big.rg(h * 2 + d2)], w=[pL], signal=(d2 == 1))
                        op("act", lambda e: e.activation(pexp[mt][:], pL[:], AF.Exp, scale=1.0 / 16), r=[pL], w=[pexp[mt]])
                    pd = pn()
                    for mt in range(2):
                        op("pe", lambda e: e.matmul(pd[:], onesb[:], pexp[mt][:], start=(mt == 0), stop=(mt == 1)), r=[onesb, pexp[mt]], w=[pd], signal=(mt == 1))
                    op("dve", lambda e: e.reciprocal(rden[:], pd[:]), r=[pd], w=[rden])
                    for d2 in range(2):
                        po = pn()
                        for mt in range(2):
                            op("pe", lambda e: e.matmul(po[:], vx[:, mt, h * 256 + d2 * 128:h * 256 + (d2 + 1) * 128], pexp[mt][:], start=(mt == 0), stop=(mt == 1)), r=[vx, pexp[mt]], w=[po], signal=(mt == 1))
                        op("dve", lambda e: e.tensor_tensor(big[:, 8 + h * 2 + d2, :], po[:], rden[:], ALU.mult), r=[po, rden], w=[big.rg(8 + h * 2 + d2)])
                for nb in range(2):
                    sl = load_w(w_xo_b, deps["w_xo"], 0, 8, nb * 512, 512)
                    for tl in range(4):
                        pb = pn()
                        for k in range(8):
                            op("pe", lambda e: e.matmul(pb[:], big[:, 8 + k, T128(tl)], sl[:, k, :], start=(k == 0), stop=(k == 7)), r=[big.rg(8 + k), sl], w=[pb], signal=(k == 7))
                        op("dve", lambda e: e.tensor_tensor(x1[:, tl, nb * 512:(nb + 1) * 512], x1[:, tl, nb * 512:(nb + 1) * 512], pb[:], ALU.add), r=[x1.rg(tl), pb], w=[x1.rg(tl)])
                if b == 0 and tb == 0:
                    for tl in range(4):
                        dump("x2_%d" % tl, x1[:, tl, :], x1.rg(tl), [128, D])
                for tl in range(4):
                    norm_A(x1[:, tl, :], x1.rg(tl), gf[:], gf, tmps[tl])
                for tl in range(4):
                    norm_B(xT[:, :, T128(tl)], xT.rg(tl), tmps[tl])
                for f4 in range(6):
                    ncol = 512 if f4 < 5 else 256
                    slg = load_w(w_gate_b, deps["w_gate"], 0, 8, f4 * 512, ncol)
                    slu = load_w(w_up_b, deps["w_up"], 0, 8, f4 * 512, ncol)
                    for fl in range(ncol // 128):
                        f = f4 * 4 + fl
                        pg = pn(); pu = pn()
                        for k in range(8):
                            op("pe", lambda e: e.matmul(pg[:], slg[:, k, fl * 128:(fl + 1) * 128], xT[:, k, :], start=(k == 0), stop=(k == 7)), r=[slg, xT], w=[pg], signal=(k == 7))
                        for k in range(8):
                            op("pe", lambda e: e.matmul(pu[:], slu[:, k, fl * 128:(fl + 1) * 128], xT[:, k, :], start=(k == 0), stop=(k == 7)), r=[slu, xT], w=[pu], signal=(k == 7))
                        s_ = sg[f % 2]
                        op("act", lambda e: e.activation(s_[:], pg[:], AF.Silu), r=[pg], w=[s_])
                        op("dve", lambda e: e.tensor_tensor(big[:, f, :], s_[:], pu[:], ALU.mult), r=[s_, pu], w=[big.rg(f)])
                for nb in range(2):
                    accs = P[2:6]
                    for kc in range(3):
                        nk = 8 if kc < 2 else 6
                        sl = load_w(w_down_b, deps["w_down"], kc * 8, nk, nb * 512, 512)
                        for tl in range(4):
                            for k in range(nk):
                                f = kc * 8 + k
                                op("pe", lambda e: e.matmul(accs[tl][:], big[:, f, T128(tl)], sl[:, k, :], start=(f == 0), stop=(f == NFT - 1)), r=[big.rg(f), sl], w=[accs[tl]], signal=(k == nk - 1))
                    for tl in range(4):
                        op("dve", lambda e: e.tensor_tensor(x1[:, tl, nb * 512:(nb + 1) * 512], x1[:, tl, nb * 512:(nb + 1) * 512], accs[tl][:], ALU.add), r=[x1.rg(tl), accs[tl]], w=[x1.rg(tl)])
                for tl in range(4):
                    tt = tb * 4 + tl
                    st, junk, _ = tmps[tl % 2]
                    ob = outb[tl % 2]
                    op("dve", lambda e: e.memset(st[:, 0:1], 0.0), w=[st])
                    op("act", lambda e: e.activation(junk[:], x1[:, tl, :], AF.Square, accum_out=st[:, 0:1]), r=[x1.rg(tl)], w=[junk, st])
                    op("act", lambda e: e.activation(st[:, 1:2], st[:, 0:1], AF.Ln, bias=cc[:, 0:1], scale=1.0 / D), r=[st, cc], w=[st])
                    op("act", lambda e: e.activation(st[:, 2:3], st[:, 1:2], AF.Exp, scale=-0.5), r=[st], w=[st])
                    op("dve", lambda e: e.scalar_tensor_tensor(ob[:], x1[:, tl, :], st[:, 2:3], gfin[:], ALU.mult, ALU.mult), r=[x1.rg(tl), st, gfin], w=[ob])
                    fw.dma("sp", out_d[b, T128(tt), :], ob[:], in_t=ob, final=True)
            del slots[2:]
            sctr[0] = 0
            fw.free(t5)
            fw.free([mixdsa])
            fw.free([mixdn])
        try:
            if stop > 0:
                for b in range(NB):
                    body(b)
        except _Stop:
            pass
        fw.finish()
    return nc, dbg_outs


def _rel_bucket(d):
    import math
    d = np.maximum(d, 0)
    lr = np.log(np.maximum(d, 16).astype(np.float32) / np.float32(16)) / np.float32(math.log(128 / 16))
    large = 16 + (lr.astype(np.float32) * np.float32(16)).astype(np.int32)
    return np.where(d < 16, d, np.minimum(large, 31))


def _host_inputs(inp):
    f = lambda a: np.ascontiguousarray(np.asarray(a, dtype=np.float32))
    i = np.arange(128)
    r, c = i[:, None], i[None, :]
    consts = np.zeros((128, NCONST), np.float32)
    consts[:, C_ID:C_ID + 128] = (r == c)
    consts[:, C_TRI:C_TRI + 128] = (r <= c)
    consts[:, C_SU:C_SU + 128] = (r > c)
    consts[:, C_SL:C_SL + 128] = (r > c)
    consts[:, C_IU:C_IU + 128] = (c >= r)
    consts[:, C_CM:C_CM + 128] = np.where(c <= r, 0.0, -1e30)
    consts[:, C_ONE:C_ONE + 128] = 1.0
    consts[:, C_LI:C_LI + 128] = (c <= r)
    rowvec = np.concatenate([
        f(inp["g_mix"])[0], f(inp["g_xattn"])[0], f(inp["g_ffn"])[0], f(inp["g_final"]), f(inp["g_mem"])[0],
        f(inp["q_norm_g"])[0], f(inp["kv_norm_g"])[0], f(inp["kidx_ln_g"])[0], f(inp["kidx_ln_b"])[0],
        f(inp["dn_norm_g"])[0], f(inp["a_log"])[0], f(inp["dt_bias"])[0], f(inp["rel_bias"])[31]])
    assert rowvec.shape[0] == NR
    rows = np.ascontiguousarray(np.broadcast_to(rowvec[None, :], (128, NR)))
    cw = f(inp["conv_w"])[0]
    convw = np.ascontiguousarray(cw.reshape(4, 12, 128).transpose(2, 1, 0).reshape(128, 48))
    rb = f(inp["rel_bias"])
    j = np.arange(256)[None, :]
    bidx = _rel_bucket(j - r)
    bt = np.ascontiguousarray(rb[bidx].transpose(0, 2, 1).reshape(128, 8 * 256))
    w_uq = f(inp["w_uq"])[0]
    w_uqT = np.ascontiguousarray(w_uq.reshape(256, 8, 64).transpose(2, 1, 0).reshape(64, 8 * 256))
    w_uk = f(inp["w_uk"])[0]
    w_ukd = np.ascontiguousarray(w_uk.transpose(1, 0, 2).reshape(64, 8 * 128))
    w_uv = f(inp["w_uv"])[0]
    w_uvT = np.ascontiguousarray(w_uv.transpose(2, 0, 1).reshape(64, 8 * 128))
    w_out = f(inp["w_out"])[0]
    w_outB = np.ascontiguousarray(w_out[512:].reshape(8, 64, 1024).transpose(1, 0, 2).reshape(64, 8 * 1024))
    shared = {
        "w_in": f(inp["w_in"])[0], "w_out": w_out, "w_xq": f(inp["w_xq"])[0], "w_xkv": f(inp["w_xkv"])[0],
        "w_xo": f(inp["w_xo"])[0], "w_gate": f(inp["w_gate"])[0], "w_up": f(inp["w_up"])[0],
        "w_down": f(inp["w_down"])[0], "w_qidx": f(inp["w_qidx"])[0], "rows": rows, "consts": consts,
        "convw": convw, "bt": bt, "w_uqT": w_uqT, "w_ukd": w_ukd, "w_uvT": w_uvT, "w_outB": w_outB,
    }
    x = f(inp["x"])
    mem = f(inp["mem"])
    maps = []
    for c in range(8):
        m = dict(shared)
        m["x"] = np.ascontiguousarray(x[NB * c:NB * (c + 1)])
        m["mem"] = np.ascontiguousarray(mem[NB * c:NB * (c + 1)])
        maps.append(m)
    return maps


_NC_CACHE = {}


def kernel(**inputs):
    maps = _host_inputs(inputs)
    if "nc" not in _NC_CACHE:
        _NC_CACHE["nc"] = build_program(dbg=False)[0]
    nc = _NC_CACHE["nc"]
    res = run_bass_kernel_spmd(nc, maps, core_ids=list(range(8)))
    return np.concatenate([np.asarray(r["out"]) for r in res.results], axis=0).astype(np.float32)
```
